# Optimizing a Trainium2 kernel written in Bass

```python
import math
import jax, jax.numpy as jnp
from jax import lax
import numpy as np

D_MODEL = 2048
BATCH = 4
SEQ = 2048
DEPTH = 4

ATTN_PATTERNS = ((128, 1), (512, 4), (2048, 16))
N_ATTN_GROUPS = len(ATTN_PATTERNS)
HEADS_PER_GROUP = 8
HEAD_DIM = 128
ATTN_QKV_WIDTH = N_ATTN_GROUPS * HEADS_PER_GROUP * HEAD_DIM
ATTN_OUT = HEADS_PER_GROUP * HEAD_DIM
BLOCK = 128

SSM_GROUP_CH = 16
SSM_STATE = 64
SSM_WIDTH = D_MODEL // 2
SSM_GROUPS = SSM_WIDTH // SSM_GROUP_CH

IN_COLS = 3 * ATTN_QKV_WIDTH + SSM_WIDTH + 2 * D_MODEL

D_FF = ((8 * D_MODEL // 3 + 255) // 256) * 256
N_EXPERTS = 8
TOP_K = 2
D_FF_EXPERT = D_FF // 2
N_DENSE = (DEPTH + 1) // 2
N_MOE = DEPTH // 2

RMS_EPS = 1e-6

kernel_name = "hybrid_dilated_attn_s5_moe_adaln"


def rmsnorm(x, g):
    xf = x.astype(jnp.float32)
    y = xf * lax.rsqrt(jnp.mean(xf * xf, axis=-1, keepdims=True) + RMS_EPS)
    return (y * g.astype(jnp.float32)).astype(x.dtype)


def modulate(h, shift, scale):
    return h * (1 + scale[:, None, :]) + shift[:, None, :]


def alibi_slopes(n_heads):
    return 2.0 ** (-8.0 * (jnp.arange(n_heads, dtype=jnp.float32) + 1.0) / n_heads)


def dilated_window_attention(q, k, v, window, dilation, slopes):
    b, s, h, e = q.shape
    n_w = window // dilation
    assert n_w <= BLOCK
    L = s // dilation
    nb = -(-L // BLOCK)
    lp = nb * BLOCK

    def to_strided(t):
        return t.astype(jnp.float32).reshape(b, L, dilation, h, e).transpose(0, 2, 1, 3, 4)

    qs, ks, vs = to_strided(q), to_strided(k), to_strided(v)
    qb = jnp.pad(qs, ((0, 0), (0, 0), (0, lp - L), (0, 0), (0, 0))).reshape(b, dilation, nb, BLOCK, h, e)

    def key_blocks(t):
        tp = jnp.pad(t, ((0, 0), (0, 0), (BLOCK, lp - L), (0, 0), (0, 0))).reshape(b, dilation, nb + 1, BLOCK, h, e)
        return jnp.concatenate([tp[:, :, :-1], tp[:, :, 1:]], axis=3)

    kb, vb = key_blocks(ks), key_blocks(vs)
    q_idx = jnp.arange(nb)[:, None] * BLOCK + jnp.arange(BLOCK)[None, :]
    k_idx = jnp.arange(nb)[:, None] * BLOCK - BLOCK + jnp.arange(2 * BLOCK)[None, :]
    dist = q_idx[:, :, None] - k_idx[:, None, :]
    valid = (dist >= 0) & (dist <= n_w) & (k_idx[:, None, :] >= 0)
    penalty = slopes[None, :, None, None] * (dist * dilation).astype(jnp.float32)[:, None]

    scores = jnp.einsum('brnqhe,brnkhe->brnhqk', qb, kb) * (e ** -0.5)
    scores = jnp.where(valid[:, None], scores - penalty, -jnp.inf)
    m = jnp.max(scores, axis=-1, keepdims=True)
    p = jnp.exp(scores - m)
    l = jnp.sum(p, axis=-1, keepdims=True)
    lse = (m + jnp.log(l))[..., 0].transpose(0, 1, 2, 4, 3)
    o = jnp.einsum('brnhqk,brnkhe->brnqhe', p, vb) / l[..., 0].transpose(0, 1, 2, 4, 3)[..., None]

    o = o.reshape(b, dilation, lp, h, e)[:, :, :L].transpose(0, 2, 1, 3, 4).reshape(b, s, h, e)
    lse = lse.reshape(b, dilation, lp, h)[:, :, :L].transpose(0, 2, 1, 3).reshape(b, s, h)
    return o, lse


def dilation_mixture_attention(q, k, v):
    slopes = alibi_slopes(HEADS_PER_GROUP)
    outs, lses = [], []
    for gi, (window, dilation) in enumerate(ATTN_PATTERNS):
        o, lse = dilated_window_attention(q[:, :, gi], k[:, :, gi], v[:, :, gi], window, dilation, slopes)
        outs.append(o)
        lses.append(lse)
    w = jax.nn.softmax(jnp.stack(lses, axis=0), axis=0)
    o = jnp.sum(w[..., None] * jnp.stack(outs, axis=0), axis=0)
    b, s = o.shape[:2]
    return o.reshape(b, s, ATTN_OUT)


def _ssm_combine(left, right):
    a_l, b_l = left
    a_r, b_r = right
    return a_l * a_r, a_r * b_l + b_r


def s5_ssm(u, a_re, a_im, log_dt, b_re, b_im, c_re, c_im, d_skip, w_glu, b_glu):
    bsz, s, _ = u.shape
    uf = u.astype(jnp.float32).reshape(bsz, s, SSM_GROUPS, SSM_GROUP_CH)
    lam = lax.complex(a_re.astype(jnp.float32), a_im.astype(jnp.float32))
    dt = jnp.exp(log_dt.astype(jnp.float32))[:, None]
    a_bar = jnp.exp(lam * dt)
    b_mat = lax.complex(b_re.astype(jnp.float32), b_im.astype(jnp.float32))
    b_bar = ((a_bar - 1.0) / lam)[:, :, None] * b_mat
    c_mat = lax.complex(c_re.astype(jnp.float32), c_im.astype(jnp.float32))
    bu = jnp.einsum('bsgc,gnc->bsgn', uf.astype(jnp.complex64), b_bar)
    a_seq = jnp.broadcast_to(a_bar, bu.shape)
    _, states = lax.associative_scan(_ssm_combine, (a_seq, bu), axis=1)
    y = jnp.einsum('bsgn,gcn->bsgc', states, c_mat).real
    y = y + d_skip.astype(jnp.float32).reshape(SSM_GROUPS, SSM_GROUP_CH) * uf
    z = jax.nn.gelu(y.reshape(bsz, s, SSM_WIDTH))
    ga, gb = jnp.split(z @ w_glu.astype(jnp.float32) + b_glu.astype(jnp.float32), 2, axis=-1)
    return (ga * jax.nn.sigmoid(gb)).astype(u.dtype)


def hybrid_mixer(h, w_in, a_re, a_im, log_dt, b_re, b_im, c_re, c_im, d_skip, w_glu, b_glu, w_branch, w_out):
    bsz, s, _ = h.shape
    proj = h @ w_in
    q, k, v, u, gates = jnp.split(
        proj, [ATTN_QKV_WIDTH, 2 * ATTN_QKV_WIDTH, 3 * ATTN_QKV_WIDTH, 3 * ATTN_QKV_WIDTH + SSM_WIDTH], axis=-1)
    hs = (bsz, s, N_ATTN_GROUPS, HEADS_PER_GROUP, HEAD_DIM)
    attn = dilation_mixture_attention(q.reshape(hs), k.reshape(hs), v.reshape(hs)).astype(h.dtype)
    ssm = s5_ssm(u, a_re, a_im, log_dt, b_re, b_im, c_re, c_im, d_skip, w_glu, b_glu)
    gate_a, gate_s = jnp.split(jax.nn.sigmoid(gates), 2, axis=-1)
    merged = gate_a * (attn @ w_branch[:ATTN_OUT]) + gate_s * (ssm @ w_branch[ATTN_OUT:])
    return merged @ w_out


def swiglu(h, w1, w3, w2):
    return (jax.nn.silu(h @ w1) * (h @ w3)) @ w2


def moe_swiglu(h, router_w, router_b, w1, w3, w2):
    logits = (h @ router_w + router_b).astype(jnp.float32)
    top_vals, top_idx = lax.top_k(logits, TOP_K)
    top_w = jax.nn.softmax(top_vals, axis=-1)
    combine = jnp.sum(jax.nn.one_hot(top_idx, N_EXPERTS, dtype=jnp.float32) * top_w[..., None], axis=-2)
    combine = combine.astype(h.dtype)
    out = jnp.zeros_like(h)
    for e in range(N_EXPERTS):
        out = out + combine[..., e:e + 1] * swiglu(h, w1[e], w3[e], w2[e])
    return out


def setup_inputs(seed: int = 0) -> dict:
    key = jax.random.key(seed)
    ks = jax.random.split(key, 32)
    f32 = jnp.float32

    def nrm(k, shape, scale):
        return jax.random.normal(k, shape, f32) * scale

    d = D_MODEL
    inp = {}
    inp["x"] = nrm(ks[0], (BATCH, SEQ, d), 1.0)
    inp["c"] = nrm(ks[1], (BATCH, d), 1.0)
    inp["ada_w"] = nrm(ks[2], (DEPTH, d, 6 * d), 0.5 * d ** -0.5)
    inp["ada_b"] = nrm(ks[3], (DEPTH, 6 * d), 0.01)
    inp["norm_mix_g"] = 1.0 + nrm(ks[4], (DEPTH, d), 0.05)
    inp["norm_ffn_g"] = 1.0 + nrm(ks[5], (DEPTH, d), 0.05)
    inp["final_norm_g"] = 1.0 + nrm(ks[6], (d,), 0.05)
    inp["w_in"] = nrm(ks[7], (DEPTH, d, IN_COLS), d ** -0.5)
    inp["ssm_a_re"] = -0.5 * jnp.exp(nrm(ks[8], (DEPTH, SSM_GROUPS, SSM_STATE), 0.05))
    inp["ssm_a_im"] = math.pi * jnp.arange(SSM_STATE, dtype=f32)[None, None, :] + nrm(ks[9], (DEPTH, SSM_GROUPS, SSM_STATE), 0.01)
    inp["ssm_log_dt"] = jax.random.uniform(ks[10], (DEPTH, SSM_GROUPS), f32, math.log(1e-3), math.log(1e-1))
    inp["ssm_b_re"] = nrm(ks[11], (DEPTH, SSM_GROUPS, SSM_STATE, SSM_GROUP_CH), (2 * SSM_GROUP_CH) ** -0.5)
    inp["ssm_b_im"] = nrm(ks[12], (DEPTH, SSM_GROUPS, SSM_STATE, SSM_GROUP_CH), (2 * SSM_GROUP_CH) ** -0.5)
    inp["ssm_c_re"] = nrm(ks[13], (DEPTH, SSM_GROUPS, SSM_GROUP_CH, SSM_STATE), SSM_STATE ** -0.5)
    inp["ssm_c_im"] = nrm(ks[14], (DEPTH, SSM_GROUPS, SSM_GROUP_CH, SSM_STATE), SSM_STATE ** -0.5)
    inp["ssm_d"] = nrm(ks[15], (DEPTH, SSM_WIDTH), 1.0)
    inp["w_glu"] = nrm(ks[16], (DEPTH, SSM_WIDTH, 2 * SSM_WIDTH), SSM_WIDTH ** -0.5)
    inp["b_glu"] = nrm(ks[17], (DEPTH, 2 * SSM_WIDTH), 0.01)
    inp["w_branch"] = nrm(ks[18], (DEPTH, ATTN_OUT + SSM_WIDTH, d), ATTN_OUT ** -0.5)
    inp["w_out"] = nrm(ks[19], (DEPTH, d, d), d ** -0.5)
    inp["ffn_w1"] = nrm(ks[20], (N_DENSE, d, D_FF), d ** -0.5)
    inp["ffn_w3"] = nrm(ks[21], (N_DENSE, d, D_FF), d ** -0.5)
    inp["ffn_w2"] = nrm(ks[22], (N_DENSE, D_FF, d), D_FF ** -0.5)
    inp["router_w"] = nrm(ks[23], (N_MOE, d, N_EXPERTS), d ** -0.5)
    inp["router_b"] = nrm(ks[24], (N_MOE, N_EXPERTS), 0.01)
    inp["moe_w1"] = nrm(ks[25], (N_MOE, N_EXPERTS, d, D_FF_EXPERT), d ** -0.5)
    inp["moe_w3"] = nrm(ks[26], (N_MOE, N_EXPERTS, d, D_FF_EXPERT), d ** -0.5)
    inp["moe_w2"] = nrm(ks[27], (N_MOE, N_EXPERTS, D_FF_EXPERT, d), D_FF_EXPERT ** -0.5)
    return inp


def reference(x, c, ada_w, ada_b, norm_mix_g, norm_ffn_g, final_norm_g, w_in,
              ssm_a_re, ssm_a_im, ssm_log_dt, ssm_b_re, ssm_b_im, ssm_c_re, ssm_c_im, ssm_d,
              w_glu, b_glu, w_branch, w_out, ffn_w1, ffn_w3, ffn_w2,
              router_w, router_b, moe_w1, moe_w3, moe_w2):
    c_act = jax.nn.silu(c)
    for layer in range(DEPTH):
        mod = c_act @ ada_w[layer] + ada_b[layer]
        sh1, sc1, g1, sh2, sc2, g2 = jnp.split(mod, 6, axis=-1)
        h = modulate(rmsnorm(x, norm_mix_g[layer]), sh1, sc1)
        mix = hybrid_mixer(h, w_in[layer], ssm_a_re[layer], ssm_a_im[layer], ssm_log_dt[layer],
                           ssm_b_re[layer], ssm_b_im[layer], ssm_c_re[layer], ssm_c_im[layer],
                           ssm_d[layer], w_glu[layer], b_glu[layer], w_branch[layer], w_out[layer])
        x = x + g1[:, None, :] * mix
        h = modulate(rmsnorm(x, norm_ffn_g[layer]), sh2, sc2)
        li = layer // 2
        if layer % 2 == 0:
            f = swiglu(h, ffn_w1[li], ffn_w3[li], ffn_w2[li])
        else:
            f = moe_swiglu(h, router_w[li], router_b[li], moe_w1[li], moe_w3[li], moe_w2[li])
        x = x + g2[:, None, :] * f
    return rmsnorm(x, final_norm_g)
```

```python
import contextlib
import math
import numpy as np
import concourse.bass as bass
import concourse.mybir as mybir
from concourse.bass_utils import run_bass_kernel_spmd

F32 = mybir.dt.float32
BF16 = mybir.dt.bfloat16
I32 = mybir.dt.int32
AF = mybir.ActivationFunctionType
ALU = mybir.AluOpType
AX = mybir.AxisListType

D = 2048
S = 2048
DC = 16
NL = 4
TB = 512
NTB = 4
NCORES = 4
QKV = 3072
INCOLS = 14336
DFF = 5632
DFE = 2816
NEXP = 8
DIL = (1, 4, 16)
KSLOT = 8
SCALE = 128.0 ** -0.5
NEGBIG = -30000.0
ARENA = 206 * 1024
USED_INPUTS = []
STATS = {}


def ssl(start, n, step):
    return slice(start, start + (n - 1) * step + 1, step)


class Ev:
    __slots__ = ("sem", "val")

    def __init__(self, sem, val):
        self.sem = sem
        self.val = val


class Buf:
    __slots__ = ("w", "r", "name")

    def __init__(self, name=""):
        self.w = None
        self.r = {}
        self.name = name


class Queue:
    def __init__(self, name, sem, slots):
        self.name = name
        self.ops = []
        self.waited = {}
        self.sem = sem
        self.count = 0
        self.slots = slots
        self.slot_count = [0] * len(slots)
        self.next = 0

    def wait(self, ev, raw=False):
        if ev is None:
            return
        if ev.sem is self.sem and (self.name == "pe" or not raw):
            return
        k = id(ev.sem)
        if self.waited.get(k, 0) >= ev.val:
            return
        self.waited[k] = ev.val
        sem, val = ev.sem, ev.val
        self.ops.append(lambda e: e.wait_ge(sem, val))


class T:
    def __init__(self, t, name=""):
        self.t = t
        self.b = Buf(name)


class Builder:
    def __init__(self, nc, st):
        self.nc = nc
        self.st = st
        mk = lambda n: st.enter_context(nc.semaphore(n))
        self.q = {}
        for name in ("pe", "act", "dve", "sp", "pool"):
            self.q[name] = Queue(name, mk("s_" + name), [mk(f"s_{name}_d{i}") for i in range(KSLOT)])

    def emit(self, qn, fn, R=(), W=(), signal=True, dma=False):
        q = self.q[qn]
        for b in R:
            q.wait(b.w, raw=True)
        for b in W:
            q.wait(b.w, raw=True)
            for ev in b.r.values():
                q.wait(ev)
        if dma:
            k = q.next
            q.next = (k + 1) % len(q.slots)
            sem = q.slots[k]
            if q.slot_count[k] > 0:
                q.wait(Ev(sem, 16 * q.slot_count[k]))
            q.slot_count[k] += 1
            val = 16 * q.slot_count[k]
            q.ops.append(lambda e: fn(e).then_inc(sem, 16))
        else:
            sem = q.sem
            val = q.count + 1
            if signal:
                q.count += 1
                q.ops.append(lambda e: fn(e).then_inc(sem, 1))
            else:
                q.ops.append(lambda e: fn(e))
        ev = Ev(sem, val)
        k = id(sem)
        for b in R:
            o = b.r.get(k)
            if o is None or o.val < val:
                b.r[k] = ev
        for b in W:
            b.w = ev
            b.r = {}
        return ev

    def barrier(self):
        evs = []
        for q in self.q.values():
            if q.count > 0:
                evs.append(Ev(q.sem, q.count))
            for k, c in enumerate(q.slot_count):
                if c > 0:
                    evs.append(Ev(q.slots[k], 16 * c))
        for q in self.q.values():
            for ev in evs:
                q.wait(ev)

    def mm(self, out, lhsT, rhs, start, stop, R, W, signal=False):
        return self.emit("pe", lambda e: e.matmul(out, lhsT, rhs, start=start, stop=stop), R, W, signal)

    def transpose(self, out, in_, ident, R, W, signal=True):
        return self.emit("pe", lambda e: e.transpose(out, in_, ident), R, W, signal)

    def dma(self, qn, out, in_, R, W):
        return self.emit(qn, lambda e: e.dma_start(out=out, in_=in_), R, W, dma=True)

    def act(self, out, in_, func, R, W, scale=None, bias=None):
        kw = {}
        if scale is not None:
            kw["scale"] = scale
        if bias is not None:
            kw["bias"] = bias
        return self.emit("act", lambda e: e.activation(out, in_, func, **kw), R, W)

    def tt(self, out, a, b, op, R, W, eng="dve"):
        return self.emit(eng, lambda e: e.tensor_tensor(out, a, b, op), R, W)

    def ts(self, out, a, s1, s2, op0, op1, R, W, eng="dve"):
        if op1 is None:
            return self.emit(eng, lambda e: e.tensor_scalar(out, a, s1, None, op0), R, W)
        return self.emit(eng, lambda e: e.tensor_scalar(out, a, s1, s2, op0, op1), R, W)

    def stt(self, out, a, scalar, b, op0, op1, R, W):
        return self.emit("dve", lambda e: e.scalar_tensor_tensor(out, a, scalar, b, op0, op1), R, W)

    def copy(self, eng, out, in_, R, W):
        if eng == "act":
            return self.emit("act", lambda e: e.activation(out, in_, AF.Copy), R, W)
        return self.emit(eng, lambda e: e.tensor_copy(out, in_), R, W)

    def memset(self, eng, ap, val, W):
        return self.emit(eng, lambda e: e.memset(ap, val), (), W)

    def init_arena(self, nbytes):
        self.arena = self.st.enter_context(self.nc.sbuf_tensor("arena", [128, nbytes], mybir.dt.uint8))
        self.arena_size = nbytes
        self.top = 0
        self.peak = 0

    def sb(self, stack, name, shape, dt):
        esz = 2 if dt == BF16 else 4
        n = 1
        for d_ in shape[1:]:
            n *= d_
        nbytes = (n * esz + 63) // 64 * 64
        off = self.top
        self.top += nbytes
        self.peak = max(self.peak, self.top)
        assert self.top <= self.arena_size, f"SBUF arena overflow allocating {name}: {self.top} > {self.arena_size}"
        v = self.arena[0:shape[0], off:off + n * esz].bitcast(dt)
        if len(shape) > 2:
            names = [f"d{i}" for i in range(len(shape) - 1)]
            pat = "p (" + " ".join(names) + ") -> p " + " ".join(names)
            v = v.rearrange(pat, **{nm: sz for nm, sz in zip(names[1:], shape[2:])})
        return T(v, name)

    @contextlib.contextmanager
    def scope(self):
        mark = self.top
        yield None
        self.top = mark

    def finish(self, block):
        eng_map = {"pe": "tensor", "act": "scalar", "dve": "vector", "sp": "sync", "pool": "gpsimd"}
        for qn, q in self.q.items():
            ops = q.ops

            def body(e, ops=ops):
                for f in ops:
                    f(e)

            getattr(block, eng_map[qn])(body)


def build_program(nl=NL, dbg=None, stop_after=None):
    nc = bass.Bass("TRN2", target_bir_lowering=False)
    st = contextlib.ExitStack()
    with st:
        _build(nc, st, nl, dbg, stop_after)
    return nc


def _build(nc, st, nl, dbg, stop_after):
    def din(name, shape, dt=F32):
        return nc.dram_tensor(name, list(shape), dt, kind="ExternalInput").ap()

    def dscr(name, shape, dt):
        return nc.dram_tensor(name, list(shape), dt, kind="Internal").ap()

    def dout(name, shape, dt=F32):
        return nc.dram_tensor(name, list(shape), dt, kind="ExternalOutput").ap()

    INSHAPES = {
        "xT": [D, S],
        "cT": [128, DC],
        "ada_w": [NL, D, 6 * D],
        "ada_bT": [NL, 128, 96],
        "gmix": [128, NL, DC],
        "gffn": [128, NL, DC],
        "gfin": [128, DC],
        "w_in": [NL, D, INCOLS],
        "a_reT": [NL, 128, 64],
        "a_imT": [NL, 128, 64],
        "log_dt": [NL, 64],
        "bx": [NL, 128, 64, 16],
        "bsw": [NL, 128, 64, 16],
        "cx": [NL, 128, 64, 16],
        "csw": [NL, 128, 64, 16],
        "ssm_d": [NL, 1024],
        "w_glu": [NL, 1024, 2048],
        "bglu": [128, NL, DC],
        "w_branch": [NL, D, D],
        "w_out": [NL, D, D],
        "ffn_w1": [2, D, DFF],
        "ffn_w3": [2, D, DFF],
        "ffn_w2": [2, DFF, D],
        "router_w": [2, D, NEXP],
        "router_bT": [2, NEXP, 1],
        "moe_w1": [2, NEXP, D, DFE],
        "moe_w3": [2, NEXP, D, DFE],
        "moe_w2": [2, NEXP, DFE, D],
    }
    _ins = {}

    def IN(name):
        if name not in _ins:
            _ins[name] = nc.dram_tensor(name, list(INSHAPES[name]), F32, kind="ExternalInput").ap()
        return _ins[name]

    outT = dout("outT", [D, S])

    XS = dscr("XS", [D, S], F32)
    QT = dscr("QT", [QKV, S], BF16)
    KT = dscr("KT", [QKV, S], BF16)
    VT = dscr("VT", [3, 16, 128, 1024], BF16)
    UT = dscr("UT", [2, 128, 8, 1024], BF16)
    GT = dscr("GT", [2 * D, S], BF16)

    dbg_outs = {}
    if dbg:
        for name, shape, dt in dbg:
            dbg_outs[name] = dout("dbg_" + name, shape, dt)

    K = Builder(nc, st)
    K.init_arena(ARENA)
    cst = None

    PS = [T(st.enter_context(nc.psum_tensor(f"ps{i}", [128, 512], F32)), f"ps{i}") for i in range(8)]
    psn = [0]

    def bank():
        p = PS[psn[0] % 8]
        psn[0] += 1
        return p

    it_i = K.sb(cst, "it_i", [128, 256], I32)
    DIST2 = K.sb(cst, "DIST2", [128, 256], F32)
    NEG2 = K.sb(cst, "NEG2", [128, 256], F32)
    ident_f = K.sb(cst, "ident_f", [128, 128], F32)
    ident_b = K.sb(cst, "ident_b", [128, 128], BF16)
    Jmat = K.sb(cst, "Jmat", [128, 128], F32)
    maskM = K.sb(cst, "maskM", [128, 128], F32)
    ones_f = K.sb(cst, "ones_f", [128, 128], F32)
    ones_b = K.sb(cst, "ones_b", [128, 128], BF16)
    sgn2 = K.sb(cst, "sgn2", [128, 1], F32)
    nsgn2 = K.sb(cst, "nsgn2", [128, 1], F32)
    halfpi = K.sb(cst, "halfpi", [128, 1], F32)
    tmpc = K.sb(cst, "tmpc", [128, 128], F32)
    cact = K.sb(cst, "cact", [128, DC, 2], F32)
    craw = K.sb(cst, "craw", [128, DC], F32)
    GMIX = K.sb(cst, "GMIX", [128, NL, DC], F32)
    GFFN = K.sb(cst, "GFFN", [128, NL, DC], F32)
    GFIN = K.sb(cst, "GFIN", [128, DC], F32)
    BGLU = K.sb(cst, "BGLU", [128, NL, DC], F32)
    MODP = K.sb(cst, "MODP", [128, 6, DC], F32)
    SA = {}

    K.emit("pool", lambda e: e.iota(it_i.t[:, 0:128], [[1, 128]], base=128, channel_multiplier=-1), (), [it_i.b])
    K.emit("pool", lambda e: e.iota(it_i.t[:, 128:256], [[1, 128]], base=0, channel_multiplier=-1), (), [it_i.b])
    K.copy("dve", DIST2.t[:], it_i.t[:], [it_i.b], [DIST2.b])
    K.ts(ident_f.t[:], DIST2.t[:, 128:256], 0.0, None, ALU.is_equal, None, [DIST2.b], [ident_f.b])
    K.copy("dve", ident_b.t[:], ident_f.t[:], [ident_f.b], [ident_b.b])
    K.ts(Jmat.t[:], DIST2.t[:, 128:256], 64.0, None, ALU.is_equal, None, [DIST2.b], [Jmat.b])
    K.ts(tmpc.t[:], DIST2.t[:, 128:256], -64.0, None, ALU.is_equal, None, [DIST2.b], [tmpc.b])
    K.tt(Jmat.t[:], Jmat.t[:], tmpc.t[:], ALU.add, [tmpc.b], [Jmat.b])
    K.ts(NEG2.t[:, 128:256], DIST2.t[:, 128:256], 0.0, NEGBIG, ALU.is_lt, ALU.mult, [DIST2.b], [NEG2.b])
    K.ts(NEG2.t[:, 0:128], DIST2.t[:, 0:128], 128.0, NEGBIG, ALU.is_gt, ALU.mult, [DIST2.b], [NEG2.b])
    K.emit("pool", lambda e: e.iota(it_i.t[:, 0:128].rearrange("p (j c) -> p j c", j=8), [[16, 8], [0, 16]],
                                    base=15, channel_multiplier=-1), (), [it_i.b])
    K.copy("dve", tmpc.t[:], it_i.t[:, 0:128], [it_i.b], [tmpc.b])
    K.ts(maskM.t[:], tmpc.t[:], 0.0, None, ALU.is_ge, None, [tmpc.b], [maskM.b])
    K.memset("dve", ones_f.t[:], 1.0, [ones_f.b])
    K.memset("dve", ones_b.t[:], 1.0, [ones_b.b])
    K.memset("dve", sgn2.t[0:64, :], 1.0, [sgn2.b])
    K.memset("dve", sgn2.t[64:128, :], -1.0, [sgn2.b])
    K.memset("dve", nsgn2.t[0:64, :], -1.0, [nsgn2.b])
    K.memset("dve", nsgn2.t[64:128, :], 1.0, [nsgn2.b])
    K.memset("dve", halfpi.t[:], math.pi / 2, [halfpi.b])
    K.dma("sp", craw.t[:], IN("cT")[:, :], (), [craw.b])
    K.dma("sp", GMIX.t[:], IN("gmix")[:, :, :], (), [GMIX.b])
    K.dma("sp", GFFN.t[:], IN("gffn")[:, :, :], (), [GFFN.b])
    K.dma("sp", GFIN.t[:], IN("gfin")[:, :], (), [GFIN.b])
    K.dma("sp", BGLU.t[:], IN("bglu")[:, :, :], (), [BGLU.b])
    K.act(cact.t[:, :, 0], craw.t[:], AF.Silu, [craw.b], [cact.b])
    K.act(cact.t[:, :, 1], craw.t[:], AF.Silu, [craw.b], [cact.b])

    def dbg_dump(name, src_ap):
        if name in dbg_outs:
            K.barrier()
            K.dma("sp", dbg_outs[name], src_ap, (), ())
            K.barrier()

    def phase_mod(l):
        with K.scope() as ps_:
            WA = [K.sb(ps_, f"modw{i}", [128, DC, 512], F32) for i in range(2)]
            MOD = K.sb(ps_, "MOD", [128, 96], F32)
            ABT = K.sb(ps_, "ABT", [128, 96], F32)
            MROW = K.sb(ps_, "MROW", [2, 6 * D], F32)
            K.dma("sp", ABT.t[:], IN("ada_bT")[l], (), [ABT.b])
            wv = IN("ada_w")[l].rearrange("(dc p) c -> p dc c", p=128)
            for blk in range(24):
                w = WA[blk % 2]
                K.dma("sp", w.t[:], wv[:, :, blk * 512:(blk + 1) * 512], (), [w.b])
                pr_ = bank()
                for dc in range(DC):
                    K.mm(pr_.t[0:2, :], cact.t[:, dc, :], w.t[:, dc, :], dc == 0, dc == DC - 1, [w.b, cact.b],
                         [pr_.b], signal=(dc == DC - 1))
                K.copy("act" if blk % 2 else "dve", MROW.t[:, blk * 512:(blk + 1) * 512], pr_.t[0:2, :], [pr_.b],
                       [MROW.b])
            pm = bank()
            for j in range(96):
                K.transpose(pm.t[:, 2 * j:2 * j + 2], MROW.t[:, j * 128:(j + 1) * 128], ident_f.t[0:2, 0:2],
                            [MROW.b, ident_f.b], [pm.b], signal=(j == 95))
            K.tt(MOD.t[:], pm.t[:, 0:192:2], ABT.t[:], ALU.add, [pm.b, ABT.b], [MOD.b])
            for which, (gsrc, base) in enumerate(((GMIX, 0), (GFFN, 3))):
                o = which * 3
                K.stt(MODP.t[:, o + 0, :], MOD.t[:, (base + 1) * 16:(base + 2) * 16], 1.0, gsrc.t[:, l, :],
                      ALU.add, ALU.mult, [MOD.b, gsrc.b], [MODP.b])
                K.copy("dve", MODP.t[:, o + 1, :], MOD.t[:, (base + 0) * 16:(base + 1) * 16], [MOD.b], [MODP.b])
                K.copy("dve", MODP.t[:, o + 2, :], MOD.t[:, (base + 2) * 16:(base + 3) * 16], [MOD.b], [MODP.b])
            K.barrier()

    def phase_norm(ps_, xsrc, HB, l, mode, router=None):
        XT = [K.sb(ps_, f"nx{i}", [128, DC, TB], F32) for i in range(2)]
        SQ = [K.sb(ps_, f"nsq{i}", [128, TB], F32) for i in range(3)]
        RS = [K.sb(ps_, f"nrs{i}", [128, TB], F32) for i in range(2)]
        TM = [K.sb(ps_, f"ntm{i}", [128, TB], F32) for i in range(3)]
        H32 = [K.sb(ps_, f"nh{i}", [128, TB], F32) for i in range(3)]
        xv = xsrc.rearrange("(dc p) t -> p dc t", p=128)
        if router is not None:
            RW, LGT, RB = router
        for tb in range(NTB):
            x = XT[tb % 2]
            tsl = slice(tb * TB, (tb + 1) * TB)
            K.dma("sp", x.t[:], xv[:, :, tsl], (), [x.b])
            pss = bank()
            for dc in range(DC):
                sq = SQ[dc % 3]
                K.act(sq.t[:], x.t[:, dc, :], AF.Square, [x.b], [sq.b])
                K.mm(pss.t[:], ones_f.t[:], sq.t[:], dc == 0, dc == DC - 1, [ones_f.b, sq.b], [pss.b],
                     signal=True)
            rs = RS[tb % 2]
            K.act(rs.t[:], pss.t[:], AF.Sqrt, [pss.b], [rs.b], scale=1.0 / D, bias=EPS_AP[0])
            K.emit("dve", lambda e, o=rs.t[:]: e.reciprocal(o, o), [rs.b], [rs.b])
            if router is not None:
                psl = bank()
            for dc in range(DC):
                tm = TM[dc % 3]
                K.tt(tm.t[:], x.t[:, dc, :], rs.t[:], ALU.mult, [x.b, rs.b], [tm.b])
                if mode == 2:
                    h = H32[dc % 3]
                    K.act(h.t[:], tm.t[:], AF.Identity, [tm.b, GFIN.b], [h.b], scale=GFIN.t[:, dc:dc + 1])
                    K.dma("sp", outT[dc * 128:(dc + 1) * 128, tsl], h.t[:], [h.b], ())
                    continue
                o = mode * 3
                if router is None:
                    K.act(HB.t[:, dc, tsl], tm.t[:], AF.Identity, [tm.b, MODP.b], [HB.b],
                          scale=MODP.t[:, o, dc:dc + 1], bias=MODP.t[:, o + 1, dc:dc + 1])
                else:
                    h = H32[dc % 3]
                    K.act(h.t[:], tm.t[:], AF.Identity, [tm.b, MODP.b], [h.b],
                          scale=MODP.t[:, o, dc:dc + 1], bias=MODP.t[:, o + 1, dc:dc + 1])
                    K.copy("dve", HB.t[:, dc, tsl], h.t[:], [h.b], [HB.b])
                    K.mm(psl.t[0:NEXP, :], RW.t[:, dc, :], h.t[:], dc == 0, dc == DC - 1, [RW.b, h.b], [psl.b],
                         signal=True)
            if router is not None:
                K.act(LGT.t[:, tsl], psl.t[0:NEXP, :], AF.Identity, [psl.b, RB.b], [LGT.b], bias=RB.t[:, 0:1])

    def phase_inproj(ps_, HB, l):
        WB = [K.sb(ps_, f"ipw{i}", [128, DC, 512], BF16) for i in range(2)]
        OF = [K.sb(ps_, f"ipo{i}", [128, S], BF16) for i in range(3)]
        OT = [K.sb(ps_, f"ipt{i}", [128, 512], BF16) for i in range(4)]
        wv = IN("w_in")[l].rearrange("(dc p) c -> p dc c", p=128)
        nblk = INCOLS // 512
        evi = [0]

        def evac(out, in_, R, W, func=None):
            evi[0] += 1
            if func is not None:
                K.act(out, in_, func, R, W)
            elif evi[0] % 2 == 0:
                K.copy("act", out, in_, R, W)
            else:
                K.copy("dve", out, in_, R, W)

        order = list(range(0, 12)) + list(range(20, 28)) + list(range(12, 20))
        ofn = 0
        otn = 0
        for bi, blk in enumerate(order):
            w = WB[bi % 2]
            c0 = blk * 512
            K.dma("pool", w.t[:], wv[:, :, c0:c0 + 512], (), [w.b])
            if c0 < 2 * QKV or c0 >= 10240:
                for ct in range(4):
                    col = c0 + ct * 128
                    of = OF[ofn % 3]
                    ofn += 1
                    for tb in range(NTB):
                        p = bank()
                        for dc in range(DC):
                            K.mm(p.t[:], w.t[:, dc, ct * 128:(ct + 1) * 128], HB.t[:, dc, tb * TB:(tb + 1) * TB],
                                 dc == 0, dc == DC - 1, [w.b, HB.b], [p.b], signal=(dc == DC - 1))
                        evac(of.t[:, tb * TB:(tb + 1) * TB], p.t[:], [p.b], [of.b],
                             func=(AF.Sigmoid if c0 >= 10240 else None))
                    if col < QKV:
                        dst = QT[col:col + 128, :]
                    elif col < 2 * QKV:
                        dst = KT[col - QKV:col - QKV + 128, :]
                    else:
                        dst = GT[col - 10240:col - 10240 + 128, :]
                    K.dma("sp", dst, of.t[:], [of.b], ())
            elif c0 < 3 * QKV:
                g = (c0 - 2 * QKV) // 1024
                cg = (c0 - 2 * QKV) % 1024
                d = DIL[g]
                nb = 16 // d
                for r in range(d):
                    for b2 in range(nb):
                        blkid = r * nb + b2
                        start = r + 128 * b2 * d
                        p = bank()
                        for dc in range(DC):
                            K.mm(p.t[:], HB.t[:, dc, ssl(start, 128, d)], w.t[:, dc, :],
                                 dc == 0, dc == DC - 1, [w.b, HB.b], [p.b], signal=(dc == DC - 1))
                        ot = OT[otn % 4]
                        otn += 1
                        evac(ot.t[:], p.t[:], [p.b], [ot.b])
                        K.dma("sp", VT[g, blkid, :, cg:cg + 512], ot.t[:], [ot.b], ())
            else:
                cu = c0 - 3 * QKV
                for kap in range(2):
                    for j in range(8):
                        start = 1024 * kap + j
                        p = bank()
                        for dc in range(DC):
                            K.mm(p.t[:], HB.t[:, dc, ssl(start, 128, 8)], w.t[:, dc, :],
                                 dc == 0, dc == DC - 1, [w.b, HB.b], [p.b], signal=(dc == DC - 1))
                        ot = OT[otn % 4]
                        otn += 1
                        evac(ot.t[:], p.t[:], [p.b], [ot.b])
                        K.dma("sp", UT[kap, :, j, cu:cu + 512], ot.t[:], [ot.b], ())

    def phase_ssm(l, ST_):
        with K.scope() as ps_:
            DUY = K.sb(ps_, "DUY", [128, 2, 8, 1024], BF16)
            UTS2 = K.sb(ps_, "UTS2", [128, 2, 64, 128], BF16)
            with K.scope():
                UTS = K.sb(ps_, "UTS", [128, 2, 8, 1024], BF16)
                DB = K.sb(ps_, "DB", [128, 1024], F32)
                for kap in range(2):
                    K.dma("sp", UTS.t[:, kap], UT[kap], (), [UTS.b])
                K.dma("sp", DB.t[:], IN("ssm_d")[l, :].partition_broadcast(128), (), [DB.b])
                for kap in range(2):
                    K.tt(DUY.t[:, kap], UTS.t[:, kap], DB.t[:].unsqueeze(1).to_broadcast([128, 8, 1024]), ALU.mult,
                         [UTS.b, DB.b], [DUY.b])
                    K.copy("act" if kap else "dve",
                           UTS2.t[:, kap].rearrange("p g (j c) -> p g j c", j=8),
                           UTS.t[:, kap].rearrange("p j (g c) -> p g j c", g=64), [UTS.b], [UTS2.b])
                K.barrier()
            pipe_scope = K.scope()
            pipe_scope.__enter__()
            def t64(name):
                return K.sb(ps_, name, [128, 64], F32)

            are, aim, ldt = t64("are"), t64("aim"), t64("ldt")
            K.dma("sp", are.t[:], IN("a_reT")[l], (), [are.b])
            K.dma("sp", aim.t[:], IN("a_imT")[l], (), [aim.b])
            K.dma("sp", ldt.t[:], IN("log_dt")[l, :].partition_broadcast(128), (), [ldt.b])
            Bx = K.sb(ps_, "Bx", [128, 64, 16], F32)
            By = K.sb(ps_, "By", [128, 64, 16], F32)
            Cx = K.sb(ps_, "Cx", [128, 64, 16], F32)
            Cy = K.sb(ps_, "Cy", [128, 64, 16], F32)
            K.dma("sp", Bx.t[:], IN("bx")[l], (), [Bx.b])
            K.dma("sp", By.t[:], IN("bsw")[l], (), [By.b])
            K.dma("sp", Cx.t[:], IN("cx")[l], (), [Cx.b])
            K.dma("sp", Cy.t[:], IN("csw")[l], (), [Cy.b])
            K.ts(By.t[:], By.t[:], nsgn2.t[:, 0:1], None, ALU.mult, None, [nsgn2.b], [By.b])
            K.ts(Cx.t[:], Cx.t[:], sgn2.t[:, 0:1], None, ALU.mult, None, [sgn2.b], [Cx.b])
            K.ts(Cy.t[:], Cy.t[:], -1.0, None, ALU.mult, None, [], [Cy.b])
            w = [t64(f"w{i}") for i in range(8)]

            def mul(o, a, b):
                K.tt(o.t[:], a.t[:], b.t[:], ALU.mult, [a.b, b.b], [o.b])

            def add(o, a, b):
                K.tt(o.t[:], a.t[:], b.t[:], ALU.add, [a.b, b.b], [o.b])

            def sub(o, a, b):
                K.tt(o.t[:], a.t[:], b.t[:], ALU.subtract, [a.b, b.b], [o.b])

            def cmul(orr, oi, ar_, ai_, br, bi):
                mul(w[4], ar_, br)
                mul(w[5], ai_, bi)
                mul(w[6], ar_, bi)
                mul(w[7], ai_, br)
                sub(orr, w[4], w[5])
                add(oi, w[6], w[7])

            dt_, ar, th, mag = t64("dt_"), t64("ar"), t64("th"), t64("mag")
            K.act(dt_.t[:], ldt.t[:], AF.Exp, [ldt.b], [dt_.b])
            mul(ar, are, dt_)
            mul(th, aim, dt_)
            cr, ci = t64("cr"), t64("ci")
            TWO_PI = 2.0 * math.pi
            kint = K.sb(ps_, "kint", [128, 64], I32)

            def reduce_pi(dst, src):
                K.ts(w[0].t[:], src.t[:], 1.0 / TWO_PI, None, ALU.mult, None, [src.b], [w[0].b])
                K.copy("dve", kint.t[:], w[0].t[:], [w[0].b], [kint.b])
                K.copy("dve", w[1].t[:], kint.t[:], [kint.b], [w[1].b])
                K.stt(w[2].t[:], w[1].t[:], -TWO_PI, src.t[:], ALU.mult, ALU.add, [w[1].b, src.b], [w[2].b])
                K.ts(w[3].t[:], w[2].t[:], math.pi, -TWO_PI, ALU.is_gt, ALU.mult, [w[2].b], [w[3].b])
                K.tt(w[2].t[:], w[2].t[:], w[3].t[:], ALU.add, [w[3].b], [w[2].b])
                K.ts(w[3].t[:], w[2].t[:], -math.pi, TWO_PI, ALU.is_lt, ALU.mult, [w[2].b], [w[3].b])
                K.tt(dst.t[:], w[2].t[:], w[3].t[:], ALU.add, [w[2].b, w[3].b], [dst.b])

            thr, phr, shf = t64("thr"), t64("phr"), t64("shf")
            reduce_pi(thr, th)
            K.act(ci.t[:], thr.t[:], AF.Sin, [thr.b], [ci.b])
            K.act(shf.t[:], thr.t[:], AF.Sin, [thr.b], [shf.b], scale=0.5)
            K.ts(phr.t[:], thr.t[:], math.pi / 2, None, ALU.add, None, [thr.b], [phr.b])
            reduce_pi(phr, phr)
            K.act(cr.t[:], phr.t[:], AF.Sin, [phr.b], [cr.b])
            em1 = t64("em1")
            K.ts(em1.t[:], ar.t[:], 1.0 / 5, 1.0, ALU.mult, ALU.add, [ar.b], [em1.b])
            for dv in (4.0, 3.0, 2.0):
                mul(w[0], em1, ar)
                K.ts(em1.t[:], w[0].t[:], 1.0 / dv, 1.0, ALU.mult, ALU.add, [w[0].b], [em1.b])
            mul(w[0], em1, ar)
            K.copy("dve", em1.t[:], w[0].t[:], [w[0].b], [em1.b])
            K.ts(mag.t[:], em1.t[:], 1.0, None, ALU.add, None, [em1.b], [mag.b])
            Ar, Ai = t64("Ar"), t64("Ai")
            mul(Ar, mag, cr)
            mul(Ai, mag, ci)
            fr, fi, nr, rden = t64("fr"), t64("fi"), t64("nr"), t64("rden")
            mul(w[0], shf, shf)
            K.ts(w[1].t[:], w[0].t[:], -2.0, None, ALU.mult, None, [w[0].b], [w[1].b])
            mul(w[0], em1, cr)
            add(nr, w[0], w[1])
            mul(w[0], are, are)
            mul(w[1], aim, aim)
            add(rden, w[0], w[1])
            K.emit("dve", lambda e: e.reciprocal(rden.t[:], rden.t[:]), [rden.b], [rden.b])
            mul(w[0], nr, are)
            mul(w[1], Ai, aim)
            add(w[2], w[0], w[1])
            mul(fr, w[2], rden)
            mul(w[0], Ai, are)
            mul(w[1], nr, aim)
            sub(w[2], w[0], w[1])
            mul(fi, w[2], rden)
            Ir, Ii = t64("Ir"), t64("Ii")
            mul(w[0], Ar, Ar)
            mul(w[1], Ai, Ai)
            add(w[2], w[0], w[1])
            K.emit("dve", lambda e: e.reciprocal(w[2].t[:], w[2].t[:]), [w[2].b], [w[2].b])
            mul(Ir, Ar, w[2])
            mul(w[3], Ai, w[2])
            K.ts(Ii.t[:], w[3].t[:], -1.0, None, ALU.mult, None, [w[3].b], [Ii.b])
            KP = [K.sb(ps_, f"KP{i}", [128, 64, 8], F32) for i in range(2)]
            KQ = [K.sb(ps_, f"KQ{i}", [128, 64, 8], F32) for i in range(2)]
            KD = [K.sb(ps_, f"KD{i}", [128, 64, 8], F32) for i in range(2)]
            RE = K.sb(ps_, "RE", [128, 64, 8], F32)
            SS = K.sb(ps_, "SS", [128, 64, 8], F32)
            pr, pi_, qr, qi = t64("pr"), t64("pi_"), t64("qr"), t64("qi")
            nr2, ni2 = t64("nr2"), t64("ni2")
            K.memset("dve", pr.t[:], 1.0, [pr.b])
            K.memset("dve", pi_.t[:], 0.0, [pi_.b])

            class V:
                def __init__(self, t, ap):
                    self.t = _Slicer(ap)
                    self.b = t.b

            class _Slicer:
                def __init__(self, ap):
                    self.ap = ap

                def __getitem__(self, k):
                    return self.ap

            for p_ in range(9):
                if p_ <= 7:
                    s_ = 7 - p_
                    cmul(V(KP[0], KP[0].t[:, :, s_]), V(KP[1], KP[1].t[:, :, s_]), pr, pi_, fr, fi)
                if p_ == 0:
                    K.copy("dve", KQ[0].t[:, :, 7], pr.t[:], [pr.b], [KQ[0].b])
                    K.copy("dve", KQ[1].t[:, :, 7], pi_.t[:], [pi_.b], [KQ[1].b])
                if p_ >= 1:
                    K.copy("dve", KD[0].t[:, :, p_ - 1], pr.t[:], [pr.b], [KD[0].b])
                    K.copy("dve", KD[1].t[:, :, p_ - 1], pi_.t[:], [pi_.b], [KD[1].b])
                if p_ < 8:
                    cmul(qr, qi, pr, pi_, Ar, Ai)
                    K.copy("dve", pr.t[:], qr.t[:], [qr.b], [pr.b])
                    K.copy("dve", pi_.t[:], qi.t[:], [qi.b], [pi_.b])
            for lev in range(8):
                K.copy("dve", RE.t[:, :, lev], pr.t[:], [pr.b], [RE.b])
                K.ts(SS.t[:, :, lev], pi_.t[:], sgn2.t[:, 0:1], None, ALU.mult, None, [pi_.b, sgn2.b], [SS.b])
                if lev < 7:
                    cmul(qr, qi, pr, pi_, pr, pi_)
                    K.copy("dve", pr.t[:], qr.t[:], [qr.b], [pr.b])
                    K.copy("dve", pi_.t[:], qi.t[:], [qi.b], [pi_.b])
            K.copy("dve", nr2.t[:], Ir.t[:], [Ir.b], [nr2.b])
            K.copy("dve", ni2.t[:], Ii.t[:], [Ii.b], [ni2.b])
            for p_ in range(1, 8):
                j = 7 - p_
                K.copy("dve", KQ[0].t[:, :, j], nr2.t[:], [nr2.b], [KQ[0].b])
                K.copy("dve", KQ[1].t[:, :, j], ni2.t[:], [ni2.b], [KQ[1].b])
                if p_ < 7:
                    cmul(qr, qi, nr2, ni2, Ir, Ii)
                    K.copy("dve", nr2.t[:], qr.t[:], [qr.b], [nr2.b])
                    K.copy("dve", ni2.t[:], qi.t[:], [qi.b], [ni2.b])

            for nm_, t_ in (("KP0", KP[0]), ("KP1", KP[1]), ("KQ0", KQ[0]), ("KQ1", KQ[1]), ("KD0", KD[0]), ("KD1", KD[1]), ("RE", RE), ("SS", SS)):
                dbg_dump(nm_, t_.t[:])
            GBN = 8
            Pg = [K.sb(ps_, f"Pg{i}", [128, 8, 16], F32) for i in range(GBN)]
            Qg = [K.sb(ps_, f"Qg{i}", [128, 8, 16], F32) for i in range(GBN)]
            Dg = [K.sb(ps_, f"Dg{i}", [128, 8, 16], F32) for i in range(GBN)]
            Mg = [K.sb(ps_, f"Mg{i}", [128, 128], BF16) for i in range(GBN)]
            PTg = [K.sb(ps_, f"PTg{i}", [128, 128], BF16) for i in range(GBN)]
            Ug = [K.sb(ps_, f"Ug{i}", [128, 256], BF16) for i in range(GBN)]
            Xg = [K.sb(ps_, f"Xg{i}", [128, 256], F32) for i in range(GBN)]
            Yg = [K.sb(ps_, f"Yg{i}", [128, 256], F32) for i in range(GBN)]
            RT2 = [[K.sb(ps_, f"RT{pp}_{i}", [128, 128], F32) for i in range(GBN)] for pp in range(2)]
            tq = [K.sb(ps_, f"tq{i}", [128, 8, 16], F32) for i in range(2)]

            def kxb(dst, kr, ki, bxm, bym, g):
                a0 = kr.t[:, g, :].unsqueeze(2).to_broadcast([128, 8, 16])
                a1 = ki.t[:, g, :].unsqueeze(2).to_broadcast([128, 8, 16])
                b0 = bxm.t[:, g, :].unsqueeze(1).to_broadcast([128, 8, 16])
                b1 = bym.t[:, g, :].unsqueeze(1).to_broadcast([128, 8, 16])
                t0 = tq[0]
                K.tt(t0.t[:], a0, b0, ALU.mult, [kr.b, bxm.b], [t0.b])
                K.tt(dst.t[:], a1, b1, ALU.mult, [ki.b, bym.b], [dst.b])
                K.tt(dst.t[:], dst.t[:], t0.t[:], ALU.add, [t0.b], [dst.b])

            def flat(t_):
                return t_.t[:].rearrange("p s c -> p (s c)")

            for g0 in range(0, 64, GBN):
                gs = list(range(g0, g0 + GBN))
                for i, g in enumerate(gs):
                    kxb(Pg[i], KP[0], KP[1], Bx, By, g)
                    kxb(Qg[i], KQ[0], KQ[1], Cx, Cy, g)
                    kxb(Dg[i], KD[0], KD[1], Cx, Cy, g)
                if g0 == 0:
                    dbg_dump("Pg0", Pg[0].t[:])
                    dbg_dump("Qg0", Qg[0].t[:])
                    dbg_dump("Dg0", Dg[0].t[:])
                for i, g in enumerate(gs):
                    p = bank()
                    K.mm(p.t[:, 0:128], flat(Pg[i]), flat(Qg[i]), True, True, [Pg[i].b, Qg[i].b], [p.b], signal=True)
                    K.tt(Mg[i].t[:], p.t[:, 0:128], maskM.t[:], ALU.mult, [p.b, maskM.b], [Mg[i].b])
                    K.transpose(p.t[:, 128:256], flat(Pg[i]), ident_f.t[:], [Pg[i].b, ident_f.b], [p.b])
                    K.copy("act", PTg[i].t[:], p.t[:, 128:256], [p.b], [PTg[i].b])
                for i, g in enumerate(gs):
                    p = bank()
                    pb = p.t[:].bitcast(BF16)
                    for kap in range(2):
                        K.transpose(pb[:, kap * 128:(kap + 1) * 128], UTS2.t[:, kap, g, :],
                                    ident_b.t[:], [UTS2.b, ident_b.b], [p.b])
                    K.copy("act", Ug[i].t[:], pb[:, 0:256], [p.b], [Ug[i].b])
                for i, g in enumerate(gs):
                    p = bank()
                    K.mm(p.t[:, 0:256], PTg[i].t[:], Ug[i].t[:], True, True, [PTg[i].b, Ug[i].b], [p.b], signal=True)
                    K.copy("act", Xg[i].t[:], p.t[:, 0:256], [p.b], [Xg[i].b])
                if g0 == 0:
                    dbg_dump("Mg0", Mg[0].t[:])
                    dbg_dump("PTg0", PTg[0].t[:])
                    dbg_dump("Ug0", Ug[0].t[:])
                    dbg_dump("Xe0", Xg[0].t[:])
                def build_rt(i, g, lev):
                    rt = RT2[lev % 2][i]
                    K.ts(rt.t[:], ident_f.t[:], RE.t[:, g, lev:lev + 1], None, ALU.mult, None,
                         [ident_f.b, RE.b], [rt.b])
                    K.stt(rt.t[:], Jmat.t[:], SS.t[:, g, lev:lev + 1], rt.t[:], ALU.mult, ALU.add,
                          [Jmat.b, SS.b, rt.b], [rt.b])

                for i, g in enumerate(gs):
                    build_rt(i, g, 0)
                for lev in range(8):
                    m = 1 << lev
                    pbs = []
                    for i, g in enumerate(gs):
                        p = bank()
                        rt = RT2[lev % 2][i]
                        K.mm(p.t[:, 0:256 - m], rt.t[:], Xg[i].t[:, 0:256 - m], True, True,
                             [rt.b, Xg[i].b], [p.b], signal=True)
                        pbs.append(p)
                    if lev < 7:
                        for i, g in enumerate(gs):
                            build_rt(i, g, lev + 1)
                    for i, g in enumerate(gs):
                        p = pbs[i]
                        K.tt(Xg[i].t[:, m:256], Xg[i].t[:, m:256], p.t[:, 0:256 - m], ALU.add, [p.b, Xg[i].b],
                             [Xg[i].b])
                if g0 == 0:
                    dbg_dump("Xs0", Xg[0].t[:])
                for i, g in enumerate(gs):
                    p = bank()
                    K.mm(p.t[:, 0:256], Mg[i].t[:], Ug[i].t[:], True, False, [Mg[i].b, Ug[i].b], [p.b])
                    K.mm(p.t[:, 1:256], flat(Dg[i]), Xg[i].t[:, 0:255], False, True, [Dg[i].b, Xg[i].b], [p.b],
                         signal=True)
                    K.copy("act", Yg[i].t[:], p.t[:, 0:256], [p.b], [Yg[i].b])
                if g0 == 0:
                    dbg_dump("Yg0", Yg[0].t[:])
                for i, g in enumerate(gs):
                    p = bank()
                    for kap in range(2):
                        K.transpose(p.t[:, kap * 128:(kap + 1) * 128], Yg[i].t[:, kap * 128:(kap + 1) * 128],
                                    ident_f.t[:], [Yg[i].b, ident_f.b], [p.b])
                    for kap in range(2):
                        dst = DUY.t[:, kap, :, 16 * g:16 * g + 16]
                        K.tt(dst, p.t[:, kap * 128:(kap + 1) * 128].rearrange("p (j c) -> p j c", j=8), dst, ALU.add,
                             [p.b], [DUY.b])
            dbg_dump("DUY", DUY.t[:])
            K.barrier()
            pipe_scope.__exit__(None, None, None)
            ZT = K.sb(ps_, "ZT", [128, 8, S], BF16)
            gelu_scope = K.scope()
            gelu_scope.__enter__()
            G1 = [K.sb(ps_, f"ge{i}", [128, 2048], F32) for i in range(2)]
            G2 = [K.sb(ps_, f"gf{i}", [128, 2048], F32) for i in range(2)]
            for kap in range(2):
                for jj in range(0, 8, 2):
                    yv = DUY.t[:, kap, jj:jj + 2, :].rearrange("p j c -> p (j c)")
                    a, b_ = G1[(jj // 2) % 2], G2[(jj // 2) % 2]
                    K.act(a.t[:], yv, AF.Square, [DUY.b], [a.b])
                    K.ts(a.t[:], a.t[:], 0.044715, 1.0, ALU.mult, ALU.add, [], [a.b])
                    K.tt(b_.t[:], a.t[:], yv, ALU.mult, [a.b, DUY.b], [b_.b])
                    K.act(a.t[:], b_.t[:], AF.Sigmoid, [b_.b], [a.b], scale=1.5957691216)
                    K.tt(yv, a.t[:], yv, ALU.mult, [a.b], [DUY.b])
            ei = 0
            for kap in range(2):
                for j in range(8):
                    for cg in range(0, 8, 4):
                        p = bank()
                        pb = p.t[:].bitcast(BF16)
                        for c4 in range(4):
                            ctile = cg + c4
                            K.transpose(pb[:, c4 * 128:(c4 + 1) * 128], DUY.t[:, kap, j, ctile * 128:(ctile + 1) * 128],
                                        ident_b.t[:], [DUY.b, ident_b.b], [p.b])
                        start = 1024 * kap + j
                        dst = ZT.t[:, cg:cg + 4, ssl(start, 128, 8)]
                        src = pb[:, 0:512].rearrange("p (c k) -> p c k", c=4)
                        ei += 1
                        K.copy("act" if ei % 2 else "dve", dst, src, [p.b], [ZT.b])
            dbg_dump("ZT", ZT.t[:])
            K.barrier()
            gelu_scope.__exit__(None, None, None)
            WG = [K.sb(ps_, f"wg{i}", [128, 8, 512], BF16) for i in range(4)]
            SG = [K.sb(ps_, f"sg{i}", [128, 512], F32) for i in range(2)]
            wv = IN("w_glu")[l].rearrange("(kt p) c -> p kt c", p=128)
            n = 0
            for cb in range(2):
                wa, wb = WG[(2 * cb) % 4], WG[(2 * cb + 1) % 4]
                K.dma("pool", wa.t[:], wv[:, :, cb * 512:(cb + 1) * 512], (), [wa.b])
                K.dma("pool", wb.t[:], wv[:, :, 1024 + cb * 512:1024 + (cb + 1) * 512], (), [wb.b])
                for ct in range(4):
                    jt = cb * 4 + ct
                    for tb in range(NTB):
                        tsl = slice(tb * TB, (tb + 1) * TB)
                        pa, pbk = bank(), bank()
                        for kt in range(8):
                            K.mm(pa.t[:], wa.t[:, kt, ct * 128:(ct + 1) * 128], ZT.t[:, kt, tsl], kt == 0, kt == 7,
                                 [wa.b, ZT.b], [pa.b], signal=(kt == 7))
                        for kt in range(8):
                            K.mm(pbk.t[:], wb.t[:, kt, ct * 128:(ct + 1) * 128], ZT.t[:, kt, tsl], kt == 0, kt == 7,
                                 [wb.b, ZT.b], [pbk.b], signal=(kt == 7))
                        sg = SG[n % 2]
                        n += 1
                        K.act(sg.t[:], pbk.t[:], AF.Sigmoid, [pbk.b, BGLU.b], [sg.b], bias=BGLU.t[:, l, 8 + jt:9 + jt])
                        K.stt(ST_.t[:, jt, tsl], pa.t[:], BGLU.t[:, l, jt:jt + 1], sg.t[:], ALU.add, ALU.mult,
                              [pa.b, sg.b, BGLU.b], [ST_.b])
            K.barrier()

    def phase_attn(l, AT_):
        with K.scope() as ps_:
            QH = [K.sb(ps_, f"qh{i}", [128, 3, S], BF16) for i in range(2)]
            KH = [K.sb(ps_, f"kh{i}", [128, 3, S], BF16) for i in range(2)]
            VH = [K.sb(ps_, f"vh{i}", [128, 3, 16, 128], BF16) for i in range(2)]
            BI = [K.sb(ps_, f"bi{i}", [128, 3, 256], F32) for i in range(2)]
            OL = [K.sb(ps_, f"ol{i}", [128, 2, S], F32) for i in range(2)]
            SBs = [K.sb(ps_, f"sb{i}", [128, 256], F32) for i in range(4)]
            PTs = [K.sb(ps_, f"pt{i}", [128, 256], BF16) for i in range(4)]
            REC = K.sb(ps_, "rec", [128, S], F32)
            un = [0]
            for h in range(8):
                qh, kh, vh, bi, ol = QH[h % 2], KH[h % 2], VH[h % 2], BI[h % 2], OL[h % 2]
                for g in range(3):
                    row = (g * 8 + h) * 128
                    K.dma("sp", qh.t[:, g, :], QT[row:row + 128, :], (), [qh.b])
                    K.dma("sp", kh.t[:, g, :], KT[row:row + 128, :], (), [kh.b])
                    K.dma("sp", vh.t[:, g], VT[g, :, :, h * 128:(h + 1) * 128].rearrange("b p e -> p b e"), (),
                          [vh.b])
                    slope = 2.0 ** (-8.0 * (h + 1) / 8.0)
                    coef = -slope * DIL[g]
                    K.stt(bi.t[:, g, :], DIST2.t[:], coef, NEG2.t[:], ALU.mult, ALU.add, [DIST2.b, NEG2.b], [bi.b])
                units = []
                for g in range(3):
                    d = DIL[g]
                    nb = 16 // d
                    for r in range(d):
                        for b2 in range(nb):
                            units.append((g, d, nb, r, b2))

                def stage_a(u):
                    g, d, nb, r, b2 = u
                    start = r + 128 * b2 * d
                    qsl = ssl(start, 128, d)
                    c0 = 128 if b2 == 0 else 0
                    psS = bank()
                    K.mm(psS.t[:, 128:256], kh.t[:, g, qsl], qh.t[:, g, qsl], True, True, [kh.b, qh.b],
                         [psS.b], signal=(b2 == 0))
                    if b2 > 0:
                        pstart = start - 128 * d
                        ksl = ssl(pstart, 128, d)
                        K.mm(psS.t[:, 0:128], kh.t[:, g, ksl], qh.t[:, g, qsl], True, True, [kh.b, qh.b],
                             [psS.b], signal=True)
                    sb_, pt = SBs[un[0] % 4], PTs[un[0] % 4]
                    un[0] += 1
                    K.stt(sb_.t[:, c0:256], psS.t[:, c0:256], SCALE, bi.t[:, g, c0:256], ALU.mult, ALU.add,
                          [psS.b, bi.b], [sb_.b])
                    K.act(pt.t[:, c0:256], sb_.t[:, c0:256], AF.Exp, [sb_.b], [pt.b])
                    return pt

                def stage_b(u, pt):
                    g, d, nb, r, b2 = u
                    blkid = r * nb + b2
                    start = r + 128 * b2 * d
                    qsl = ssl(start, 128, d)
                    po = bank()
                    if b2 > 0:
                        K.mm(po.t[:, 0:128], vh.t[:, g, blkid - 1, :], pt.t[:, 0:128], True, False,
                             [vh.b, pt.b], [po.b])
                    K.mm(po.t[:, 0:128], vh.t[:, g, blkid, :], pt.t[:, 128:256], b2 == 0, True, [vh.b, pt.b],
                         [po.b])
                    if b2 > 0:
                        K.mm(po.t[:, 128:256], ones_b.t[:], pt.t[:, 0:128], True, False, [ones_b.b, pt.b],
                             [po.b])
                    K.mm(po.t[:, 128:256], ones_b.t[:], pt.t[:, 128:256], b2 == 0, True, [ones_b.b, pt.b],
                         [po.b], signal=True)
                    src = po.t[:, 0:256].rearrange("p (a q) -> p a q", a=2)
                    dst = ol.t[:, :, qsl]
                    if g == 0:
                        K.copy("dve", dst, src, [po.b], [ol.b])
                    else:
                        K.tt(dst, src, dst, ALU.add, [po.b, ol.b], [ol.b])

                pend = []
                for u in units:
                    pend.append((u, stage_a(u)))
                    if len(pend) > 2:
                        stage_b(*pend.pop(0))
                while pend:
                    stage_b(*pend.pop(0))
                K.emit("dve", lambda e, o=REC.t[:], i_=ol.t[:, 1, :]: e.reciprocal(o, i_), [ol.b], [REC.b])
                K.tt(AT_.t[:, h, :], ol.t[:, 0, :], REC.t[:], ALU.mult, [ol.b, REC.b], [AT_.b])
            K.barrier()

    def phase_mixout(l, xsrc, ST_, AT_):
        with K.scope() as ps_:
            MB = K.sb(ps_, "MB", [128, DC, S], BF16)
            WB = [K.sb(ps_, f"mw{i}", [128, DC, 512], BF16) for i in range(2)]
            GA = [K.sb(ps_, f"ga{i}", [128, S], BF16) for i in range(2)]
            GS_ = [K.sb(ps_, f"gs{i}", [128, S], BF16) for i in range(2)]
            T1 = [K.sb(ps_, f"t1{i}", [128, TB], F32) for i in range(2)]
            T2 = [K.sb(ps_, f"t2{i}", [128, TB], F32) for i in range(2)]
            XTL = [K.sb(ps_, f"xt{i}", [128, TB], F32) for i in range(4)]
            wvb = IN("w_branch")[l].rearrange("(kt p) c -> p kt c", p=128)
            wvo = IN("w_out")[l].rearrange("(kt p) c -> p kt c", p=128)
            xo = XS.rearrange("(dc p) t -> p dc t", p=128)
            wsteps = [(wvb, cb) for cb in range(4)] + [(wvo, cb) for cb in range(4)]
            wslot = {}
            wiss = [0]

            def wissue(n_):
                while wiss[0] < min(n_, len(wsteps)):
                    wv_, cb_ = wsteps[wiss[0]]
                    w_ = WB[wiss[0] % 2]
                    K.dma("pool", w_.t[:], wv_[:, :, cb_ * 512:(cb_ + 1) * 512], (), [w_.b])
                    wslot[wiss[0]] = w_
                    wiss[0] += 1

            n = 0
            for cb in range(4):
                wissue(cb + 2)
                w = wslot[cb]
                for ct in range(4):
                    dt = cb * 4 + ct
                    ga, gs = GA[dt % 2], GS_[dt % 2]
                    K.dma("sp", ga.t[:], GT[dt * 128:(dt + 1) * 128, :], (), [ga.b])
                    K.dma("sp", gs.t[:], GT[D + dt * 128:D + (dt + 1) * 128, :], (), [gs.b])
                    for tb in range(NTB):
                        tsl = slice(tb * TB, (tb + 1) * TB)
                        pa, pb = bank(), bank()
                        for kt in range(8):
                            K.mm(pa.t[:], w.t[:, kt, ct * 128:(ct + 1) * 128], AT_.t[:, kt, tsl], kt == 0, kt == 7,
                                 [w.b, AT_.b], [pa.b], signal=(kt == 7))
                        for kt in range(8):
                            K.mm(pb.t[:], w.t[:, 8 + kt, ct * 128:(ct + 1) * 128], ST_.t[:, kt, tsl], kt == 0, kt == 7,
                                 [w.b, ST_.b], [pb.b], signal=(kt == 7))
                        t1, t2 = T1[n % 2], T2[n % 2]
                        n += 1
                        K.tt(t1.t[:], pa.t[:], ga.t[:, tsl], ALU.mult, [pa.b, ga.b], [t1.b])
                        K.tt(t2.t[:], pb.t[:], gs.t[:, tsl], ALU.mult, [pb.b, gs.b], [t2.b])
                        K.tt(MB.t[:, dt, tsl], t1.t[:], t2.t[:], ALU.add, [t1.b, t2.b], [MB.b])
            n = 0
            for cb in range(4):
                wissue(4 + cb + 2)
                w = wslot[4 + cb]
                for ct in range(4):
                    dt = cb * 4 + ct
                    for tb in range(NTB):
                        tsl = slice(tb * TB, (tb + 1) * TB)
                        xt = XTL[n % 4]
                        n += 1
                        p = bank()
                        for kt in range(DC):
                            K.mm(p.t[:], w.t[:, kt, ct * 128:(ct + 1) * 128], MB.t[:, kt, tsl], kt == 0, kt == DC - 1,
                                 [w.b, MB.b], [p.b], signal=(kt == DC - 1))
                        K.ts(xt.t[:], p.t[:], MODP.t[:, 2, dt:dt + 1], None, ALU.mult, None, [p.b, MODP.b], [xt.b])
                        K.emit("pool", lambda e_, o=xo[:, dt, tsl], i_=xt.t[:]: e_.dma_start(out=o, in_=i_,
                                                                                           accum_op=ALU.add),
                               [xt.b], (), dma=True)
            K.barrier()

    def phase_ffn(l):
        li = l // 2
        moe = (l % 2 == 1)
        with K.scope() as ps_:
            HB = K.sb(ps_, "HB2", [128, DC, S], BF16)
            if not moe:
                with K.scope() as pn_:
                    phase_norm(pn_, XS, HB, l, 1, None)
                    K.barrier()
            else:
                CMB = K.sb(ps_, "CMB", [128, 16, NEXP], F32)
                with K.scope():
                    RW = K.sb(ps_, "RW", [128, DC, NEXP], F32)
                    LGT = K.sb(ps_, "LGT", [NEXP, S], F32)
                    RB = K.sb(ps_, "RB", [NEXP, 1], F32)
                    K.dma("sp", RW.t[:], IN("router_w")[li].rearrange("(dc p) e -> p dc e", p=128), (), [RW.b])
                    K.dma("sp", RB.t[:], IN("router_bT")[li], (), [RB.b])
                    with K.scope() as pn_:
                        phase_norm(pn_, XS, HB, l, 1, (RW, LGT, RB))
                        K.barrier()
                    LG = K.sb(ps_, "LG", [128, 16, NEXP], F32)
                    LG2 = K.sb(ps_, "LG2", [128, 16, NEXP], F32)
                    E1 = K.sb(ps_, "E1", [128, 16, NEXP], F32)
                    E2 = K.sb(ps_, "E2", [128, 16, NEXP], F32)
                    M1 = K.sb(ps_, "M1", [128, 16], F32)
                    M2 = K.sb(ps_, "M2", [128, 16], F32)
                    W1_ = K.sb(ps_, "W1_", [128, 16], F32)
                    W2_ = K.sb(ps_, "W2_", [128, 16], F32)
                    p = bank()
                    for tt in range(16):
                        K.transpose(p.t[:, tt * 8:(tt + 1) * 8], LGT.t[:, tt * 128:(tt + 1) * 128],
                                    ident_f.t[0:NEXP, 0:NEXP], [LGT.b, ident_f.b], [p.b])
                    K.copy("dve", LG.t[:].rearrange("p a e -> p (a e)"), p.t[:, 0:128], [p.b], [LG.b])
                    bc = lambda m: m.t[:].unsqueeze(2).to_broadcast([128, 16, NEXP])
                    K.emit("dve", lambda e: e.tensor_reduce(M1.t[:], LG.t[:], AX.X, ALU.max), [LG.b], [M1.b])
                    K.tt(E1.t[:], LG.t[:], bc(M1), ALU.is_equal, [LG.b, M1.b], [E1.b])
                    K.stt(LG2.t[:], E1.t[:], -1e30, LG.t[:], ALU.mult, ALU.add, [E1.b, LG.b], [LG2.b])
                    K.emit("dve", lambda e: e.tensor_reduce(M2.t[:], LG2.t[:], AX.X, ALU.max), [LG2.b], [M2.b])
                    K.tt(E2.t[:], LG2.t[:], bc(M2), ALU.is_equal, [LG2.b, M2.b], [E2.b])
                    K.tt(W1_.t[:], M1.t[:], M2.t[:], ALU.subtract, [M1.b, M2.b], [W1_.b])
                    K.act(W2_.t[:], W1_.t[:], AF.Sigmoid, [W1_.b], [W2_.b], scale=-1.0)
                    K.act(W1_.t[:], W1_.t[:], AF.Sigmoid, [W1_.b], [W1_.b])
                    K.tt(E1.t[:], E1.t[:], bc(W1_), ALU.mult, [E1.b, W1_.b], [E1.b])
                    K.tt(E2.t[:], E2.t[:], bc(W2_), ALU.mult, [E2.b, W2_.b], [E2.b])
                    K.tt(CMB.t[:], E1.t[:], E2.t[:], ALU.add, [E1.b, E2.b], [CMB.b])
                    dbg_dump("CMB", CMB.t[:])
                    K.barrier()
                DG = [K.sb(ps_, f"dg{i}", [128, 128], F32) for i in range(2)]
                CB = [K.sb(ps_, f"cb{i}", [128, S], BF16) for i in range(2)]
            G = 11
            HID = K.sb(ps_, "HID", [128, G, S], BF16)
            W13 = [K.sb(ps_, f"w13{i}", [128, 2, DC, 256], BF16) for i in range(2)]
            W2S = [K.sb(ps_, f"w2s{i}", [128, G, 512], BF16) for i in range(2)]
            T1 = [K.sb(ps_, f"f1{i}", [128, TB], BF16) for i in range(3)]
            T2 = [K.sb(ps_, f"f2{i}", [128, TB], F32) for i in range(3)]
            XTL = [K.sb(ps_, f"fx{i}", [128, TB], F32) for i in range(4)]
            xo = XS.rearrange("(dc p) t -> p dc t", p=128)
            XB = [[Buf(f"x{dt}_{tb}") for tb in range(NTB)] for dt in range(DC)]
            if moe:
                groups = [(IN("moe_w1")[li, e], IN("moe_w3")[li, e], IN("moe_w2")[li, e], gi * G, e) for e in range(NEXP) for gi in range(2)]
            else:
                groups = [(IN("ffn_w1")[li], IN("ffn_w3")[li], IN("ffn_w2")[li], gi * G, None) for gi in range(4)]
            tn = 0
            xn = 0
            cur_e = None
            steps = []
            for gidx in range(len(groups)):
                ftl = 0
                while ftl < G:
                    nt = min(2, G - ftl)
                    steps.append(("w13", gidx, ftl, nt))
                    ftl += nt
                for db in range(4):
                    steps.append(("w2", gidx, db, 0))
            slot_of = {}
            cnt = {"w13": 0, "w2": 0}
            issued = [0]

            def issue_upto(n_):
                while issued[0] < min(n_, len(steps)):
                    kind, gidx, a_, b__ = steps[issued[0]]
                    w1, w3, w2, ft0, e_ = groups[gidx]
                    if kind == "w13":
                        w = W13[cnt["w13"] % 2]
                        cnt["w13"] += 1
                        c0 = (ft0 + a_) * 128
                        w1v = w1.rearrange("(dc p) c -> p dc c", p=128)
                        w3v = w3.rearrange("(dc p) c -> p dc c", p=128)
                        K.dma("pool", w.t[:, 0, :, 0:b__ * 128], w1v[:, :, c0:c0 + b__ * 128], (), [w.b])
                        K.dma("pool", w.t[:, 1, :, 0:b__ * 128], w3v[:, :, c0:c0 + b__ * 128], (), [w.b])
                    else:
                        w = W2S[cnt["w2"] % 2]
                        cnt["w2"] += 1
                        w2v = w2[ft0 * 128:(ft0 + G) * 128, :].rearrange("(ft p) c -> p ft c", p=128)
                        K.dma("pool", w.t[:], w2v[:, :, a_ * 512:(a_ + 1) * 512], (), [w.b])
                    slot_of[issued[0]] = w
                    issued[0] += 1

            for si, (kind, gidx, a_, b__) in enumerate(steps):
                (w1, w3, w2, ft0, e) = groups[gidx]
                if moe and e != cur_e:
                    cur_e = e
                    cb_ = CB[e % 2]
                    for tt in range(16):
                        dg = DG[tt % 2]
                        K.ts(dg.t[:], ident_f.t[:], CMB.t[:, tt, e:e + 1], None, ALU.mult, None, [ident_f.b, CMB.b],
                             [dg.b])
                        if tt % 4 == 0:
                            p = bank()
                        K.mm(p.t[:, (tt % 4) * 128:(tt % 4 + 1) * 128], ones_f.t[:], dg.t[:], True, True,
                             [ones_f.b, dg.b], [p.b], signal=True)
                        if tt % 4 == 3:
                            K.copy("act", cb_.t[:, (tt - 3) * 128:(tt + 1) * 128], p.t[:], [p.b], [cb_.b])
                issue_upto(si + 2)
                w = slot_of[si]
                if kind == "w13":
                    ftl, nt = a_, b__
                    for t_ in range(nt):
                        for tb in range(NTB):
                            tsl = slice(tb * TB, (tb + 1) * TB)
                            p1, p3 = bank(), bank()
                            for dc in range(DC):
                                K.mm(p1.t[:], w.t[:, 0, dc, t_ * 128:(t_ + 1) * 128], HB.t[:, dc, tsl], dc == 0,
                                     dc == DC - 1, [w.b, HB.b], [p1.b], signal=(dc == DC - 1))
                            for dc in range(DC):
                                K.mm(p3.t[:], w.t[:, 1, dc, t_ * 128:(t_ + 1) * 128], HB.t[:, dc, tsl], dc == 0,
                                     dc == DC - 1, [w.b, HB.b], [p3.b], signal=(dc == DC - 1))
                            t1 = T1[tn % 3]
                            t2 = T2[tn % 3]
                            tn += 1
                            K.act(t1.t[:], p1.t[:], AF.Silu, [p1.b], [t1.b])
                            if moe:
                                K.tt(t2.t[:], p3.t[:], cb_.t[:, tsl], ALU.mult, [p3.b, cb_.b], [t2.b])
                                K.tt(HID.t[:, ftl + t_, tsl], t2.t[:], t1.t[:], ALU.mult, [t2.b, t1.b], [HID.b])
                            else:
                                K.tt(HID.t[:, ftl + t_, tsl], p3.t[:], t1.t[:], ALU.mult, [p3.b, t1.b], [HID.b])
                else:
                    db = a_
                    ws = w
                    for ct in range(4):
                        dt = db * 4 + ct
                        for tb in range(NTB):
                            tsl = slice(tb * TB, (tb + 1) * TB)
                            xt = XTL[xn % 4]
                            xn += 1
                            xb = XB[dt][tb]
                            p = bank()
                            for ft in range(G):
                                K.mm(p.t[:], ws.t[:, ft, ct * 128:(ct + 1) * 128], HID.t[:, ft, tsl], ft == 0,
                                     ft == G - 1, [ws.b, HID.b], [p.b], signal=(ft == G - 1))
                            K.ts(xt.t[:], p.t[:], MODP.t[:, 5, dt:dt + 1], None, ALU.mult, None, [p.b, MODP.b],
                                 [xt.b])
                            K.emit("pool", lambda e_, o=xo[:, dt, tsl], i_=xt.t[:]: e_.dma_start(out=o, in_=i_,
                                                                                               accum_op=ALU.add),
                                   [xt.b], [xb], dma=True)
            K.barrier()

    epsT = K.sb(cst, "epsT", [128, 1], F32)
    K.memset("dve", epsT.t[:], 1e-6, [epsT.b])
    EPS_AP = [epsT.t[:, 0:1]]
    K.barrier()


    for dc_ in range(DC):
        K.dma("sp", XS[dc_ * 128:(dc_ + 1) * 128, :], IN("xT")[dc_ * 128:(dc_ + 1) * 128, :], (), ())
    K.barrier()
    done = False
    for l in range(nl):
        xsrc = XS
        phase_mod(l)
        if stop_after == "mod":
            dbg_dump("MODP", MODP.t[:])
            break
        with K.scope() as pl_:
            HB = K.sb(pl_, "HB", [128, DC, S], BF16)
            with K.scope() as pn_:
                phase_norm(pn_, xsrc, HB, l, 0)
                K.barrier()
            if stop_after == "norm":
                dbg_dump("HB", HB.t[:])
                break
            with K.scope() as pp_:
                phase_inproj(pp_, HB, l)
                K.barrier()
        if stop_after == "inproj":
            break
        with K.scope() as pl_:
            ST_ = K.sb(pl_, "ST_", [128, 8, S], BF16)
            phase_ssm(l, ST_)
            if stop_after == "ssm":
                dbg_dump("ST", ST_.t[:])
                break
            AT_ = K.sb(pl_, "AT_", [128, 8, S], BF16)
            phase_attn(l, AT_)
            if stop_after == "attn":
                dbg_dump("AT", AT_.t[:])
                dbg_dump("ST", ST_.t[:])
                break
            phase_mixout(l, xsrc, ST_, AT_)
        if stop_after == "mixout":
            break
        phase_ffn(l)
        if stop_after == "ffn":
            break
    else:
        done = True
    if done and nl == NL and stop_after is None:
        with K.scope() as pn_:
            phase_norm(pn_, XS, None, 0, 2)
            K.barrier()

    for name, src in (("QT", QT), ("KT", KT), ("VT", VT), ("UT", UT), ("GT", GT), ("XS", XS)):
        if name in dbg_outs:
            K.barrier()
            K.dma("sp", dbg_outs[name], src, (), ())
    K.barrier()
    block = st.enter_context(nc.Block())
    K.finish(block)
    USED_INPUTS[:] = list(_ins.keys())
    STATS['arena_peak'] = K.peak
    STATS['ninst'] = {qn: len(q.ops) for qn, q in K.q.items()}


def prep_shared(inp):
    f = lambda a: np.ascontiguousarray(np.asarray(a, dtype=np.float32))
    sh = {}
    sh["ada_w"] = f(inp["ada_w"])
    sh["ada_bT"] = f(np.asarray(inp["ada_b"]).reshape(NL, 96, 128).transpose(0, 2, 1))
    sh["gmix"] = f(np.asarray(inp["norm_mix_g"]).reshape(NL, DC, 128).transpose(2, 0, 1))
    sh["gffn"] = f(np.asarray(inp["norm_ffn_g"]).reshape(NL, DC, 128).transpose(2, 0, 1))
    sh["gfin"] = f(np.asarray(inp["final_norm_g"]).reshape(DC, 128).T)
    sh["w_in"] = f(inp["w_in"])
    are = np.asarray(inp["ssm_a_re"]).transpose(0, 2, 1)
    aim = np.asarray(inp["ssm_a_im"]).transpose(0, 2, 1)
    sh["a_reT"] = f(np.concatenate([are, are], axis=1))
    sh["a_imT"] = f(np.concatenate([aim, aim], axis=1))
    sh["log_dt"] = f(inp["ssm_log_dt"])
    bre = np.asarray(inp["ssm_b_re"]).transpose(0, 2, 1, 3)
    bim = np.asarray(inp["ssm_b_im"]).transpose(0, 2, 1, 3)
    sh["bx"] = f(np.concatenate([bre, bim], axis=1))
    sh["bsw"] = f(np.concatenate([bim, bre], axis=1))
    cre = np.asarray(inp["ssm_c_re"]).transpose(0, 3, 1, 2)
    cim = np.asarray(inp["ssm_c_im"]).transpose(0, 3, 1, 2)
    sh["cx"] = f(np.concatenate([cre, cim], axis=1))
    sh["csw"] = f(np.concatenate([cim, cre], axis=1))
    sh["ssm_d"] = f(inp["ssm_d"])
    sh["w_glu"] = f(inp["w_glu"])
    sh["bglu"] = f(np.asarray(inp["b_glu"]).reshape(NL, DC, 128).transpose(2, 0, 1))
    sh["w_branch"] = f(inp["w_branch"])
    sh["w_out"] = f(inp["w_out"])
    sh["ffn_w1"] = f(inp["ffn_w1"])
    sh["ffn_w3"] = f(inp["ffn_w3"])
    sh["ffn_w2"] = f(inp["ffn_w2"])
    sh["router_w"] = f(inp["router_w"])
    sh["router_bT"] = f(np.asarray(inp["router_b"]).reshape(2, NEXP, 1))
    sh["moe_w1"] = f(inp["moe_w1"])
    sh["moe_w3"] = f(inp["moe_w3"])
    sh["moe_w2"] = f(inp["moe_w2"])
    return sh


def per_core(inp, b):
    x = np.asarray(inp["x"], dtype=np.float32)
    c = np.asarray(inp["c"], dtype=np.float32)
    return {"xT": np.ascontiguousarray(x[b].T), "cT": np.ascontiguousarray(c[b].reshape(DC, 128).T)}


def kernel(**inputs):
    sh = prep_shared(inputs)
    in_maps = []
    for b in range(NCORES):
        m = dict(sh)
        m.update(per_core(inputs, b))
        in_maps.append(m)
    nc = build_program()
    in_maps = [{k: m[k] for k in USED_INPUTS} for m in in_maps]
    res = run_bass_kernel_spmd(nc, in_maps, core_ids=list(range(NCORES)))
    out = np.stack([np.ascontiguousarray(res.results[b]["outT"].T) for b in range(NCORES)], axis=0)
    return out.astype(np.float32)
```
